# Optimizing a Trainium2 kernel written in Bass

```python
import jax, jax.numpy as jnp
from jax import lax
import numpy as np

D_MODEL = 2048
BATCH = 2
SEQ = 4096
DEPTH = 2

SWA_HEADS = 8
SWA_KV_HEADS = 2
SWA_HEAD_DIM = 64
SWA_WINDOW = 128
M_HEADS = 4
M_QK_DIM = 64
M_V_DIM = 128
M_CHUNK = 64
M_CONV = 4
C_HEADS = 8
C_NOPE_DIM = 128
C_ROPE_DIM = 64
C_V_DIM = 128
C_Q_LORA = 512
C_KV_LORA = 256
C_Q_BLOCK = 128
ROPE_THETA = 10000.0
N_EXPERTS = 32
TOP_K = 4
D_EXPERT = D_MODEL
SWIGLU_LIMIT = 7.0
SWIGLU_ALPHA = 1.702
MOE_BLOCK = 256
DN_ALPHA = (2 * DEPTH) ** 0.25
DN_BETA = (8 * DEPTH) ** -0.25
LN_EPS = 1e-5
RMS_EPS = 1e-6

A_Q = SWA_HEADS * SWA_HEAD_DIM
A_KV = SWA_KV_HEADS * SWA_HEAD_DIM
B_QK = M_HEADS * M_QK_DIM
B_V = M_HEADS * M_V_DIM
C_OUT = C_HEADS * C_V_DIM
IN_WIDTHS = (A_Q, A_KV, A_KV, B_QK, B_QK, B_V, B_V, 2 * M_HEADS, C_Q_LORA, C_KV_LORA, C_ROPE_DIM)
N_IN = sum(IN_WIDTHS)
D_MIX = A_Q + B_V + C_OUT

kernel_name = "hymba_style_swa_mlstm_mla_moe_deepnorm"

F32 = jnp.float32


def layer_norm(x, g, b):
    xf = x.astype(F32)
    mu = xf.mean(-1, keepdims=True)
    var = jnp.mean(jnp.square(xf - mu), -1, keepdims=True)
    return ((xf - mu) * lax.rsqrt(var + LN_EPS) * g.astype(F32) + b.astype(F32)).astype(x.dtype)


def rms_norm(x, g):
    xf = x.astype(F32)
    y = xf * lax.rsqrt(jnp.mean(jnp.square(xf), -1, keepdims=True) + RMS_EPS)
    return (y * g.astype(F32)).astype(x.dtype)


def head_layer_norm(h, g):
    mu = h.mean(-1, keepdims=True)
    var = jnp.mean(jnp.square(h - mu), -1, keepdims=True)
    return (h - mu) * lax.rsqrt(var + LN_EPS) * g.astype(F32).reshape(h.shape[-2:])


def rope_tables(seq, dim):
    inv = 1.0 / (ROPE_THETA ** (jnp.arange(0, dim, 2, dtype=F32) / dim))
    ang = jnp.arange(seq, dtype=F32)[:, None] * inv[None, :]
    return jnp.cos(ang), jnp.sin(ang)


def apply_rope(x, cos, sin):
    x1, x2 = jnp.split(x.astype(F32), 2, axis=-1)
    return jnp.concatenate([x1 * cos - x2 * sin, x2 * cos + x1 * sin], -1).astype(x.dtype)


def split_columns(p):
    outs, start = [], 0
    for w in IN_WIDTHS:
        outs.append(p[..., start:start + w])
        start += w
    return outs


def causal_depthwise_conv(x, w, bias):
    k = w.shape[0]
    y = lax.conv_general_dilated(x, w[:, None, :].astype(x.dtype), window_strides=(1,),
                                 padding=((k - 1, 0),), dimension_numbers=('NWC', 'WIO', 'NWC'),
                                 feature_group_count=x.shape[-1])
    return y + bias.astype(x.dtype)


def sliding_window_attention(q, k, v, sinks):
    b, s, _, dh = q.shape
    w = SWA_WINDOW
    nb = s // w
    g = SWA_HEADS // SWA_KV_HEADS
    qb = q.reshape(b, nb, w, SWA_KV_HEADS, g, dh)

    def with_prev(t):
        tb = t.reshape(b, nb, w, SWA_KV_HEADS, dh)
        prev = jnp.pad(tb, ((0, 0), (1, 0), (0, 0), (0, 0), (0, 0)))[:, :-1]
        return jnp.concatenate([prev, tb], axis=2)

    kk, vv = with_prev(k), with_prev(v)
    scores = jnp.einsum('bnqhgd,bnkhd->bnhgqk', qb, kk, preferred_element_type=F32) * (dh ** -0.5)
    q_idx = jnp.arange(w)[:, None] + w
    k_idx = jnp.arange(2 * w)[None, :]
    rel = q_idx - k_idx
    band = (rel >= 0) & (rel < w)
    has_prev = (jnp.arange(nb)[:, None, None] > 0) | (k_idx[None] >= w)
    mask = band[None] & has_prev
    scores = jnp.where(mask[None, :, None, None], scores, -jnp.inf)
    sink = sinks.astype(F32).reshape(1, 1, SWA_KV_HEADS, g, 1, 1)
    sink = jnp.broadcast_to(sink, scores.shape[:-1] + (1,))
    probs = jax.nn.softmax(jnp.concatenate([scores, sink], -1), axis=-1)[..., :-1]
    out = jnp.einsum('bnhgqk,bnkhd->bnqhgd', probs.astype(v.dtype), vv)
    return out.reshape(b, s, SWA_HEADS * dh)


def mlstm(q, k, v, i_pre, f_pre):
    b, s, h, dqk = q.shape
    L = M_CHUNK
    nc = s // L

    def chunks(t):
        t = t.reshape((b, nc, L, h) + t.shape[3:])
        return jnp.moveaxis(t, (1, 3), (0, 2))

    qc = chunks(q.astype(F32)) * (dqk ** -0.5)
    kc = chunks(k.astype(F32))
    vc = chunks(v.astype(F32))
    ic = chunks(i_pre.astype(F32))
    lfc = chunks(jax.nn.log_sigmoid(f_pre.astype(F32)))
    causal = jnp.tril(jnp.ones((L, L), bool))

    def step(carry, xs):
        C, n, m = carry
        qj, kj, vj, ij, lf = xs
        bcum = jnp.cumsum(lf, -1)
        D = bcum[..., :, None] - bcum[..., None, :] + ij[..., None, :]
        D = jnp.where(causal, D, -jnp.inf)
        m_inter = bcum + m[..., None]
        m_j = jnp.maximum(m_inter, D.max(-1))
        w_intra = jnp.exp(D - m_j[..., None])
        w_inter = jnp.exp(m_inter - m_j)
        qk = jnp.einsum('bhjd,bhsd->bhjs', qj, kj) * w_intra
        num = jnp.einsum('bhjs,bhsv->bhjv', qk, vj) + w_inter[..., None] * jnp.einsum('bhvd,bhjd->bhjv', C, qj)
        nq = qk.sum(-1) + w_inter * jnp.einsum('bhd,bhjd->bhj', n, qj)
        den = jnp.maximum(jnp.abs(nq), jnp.exp(-m_j))
        hj = num / den[..., None]
        m_new = m_j[..., -1]
        w_s = jnp.exp(bcum[..., -1:] - bcum + ij - m_new[..., None])
        w_c = jnp.exp(bcum[..., -1] + m - m_new)
        C_new = w_c[..., None, None] * C + jnp.einsum('bhs,bhsv,bhsd->bhvd', w_s, vj, kj)
        n_new = w_c[..., None] * n + jnp.einsum('bhs,bhsd->bhd', w_s, kj)
        return (C_new, n_new, m_new), hj

    init = (jnp.zeros((b, h, v.shape[-1], dqk), F32), jnp.zeros((b, h, dqk), F32), jnp.zeros((b, h), F32))
    _, hs = lax.scan(step, init, (qc, kc, vc, ic, lfc))
    return jnp.moveaxis(hs, (0, 2), (1, 3)).reshape(b, s, h, v.shape[-1])


def mla(c_q, c_kv, k_rope, q_norm_g, w_uq, kv_norm_g, w_ukv, cos, sin):
    b, s, _ = c_q.shape
    q = (rms_norm(c_q, q_norm_g) @ w_uq).reshape(b, s, C_HEADS, C_NOPE_DIM + C_ROPE_DIM)
    q_nope = q[..., :C_NOPE_DIM]
    q_rope = apply_rope(q[..., C_NOPE_DIM:], cos[:, None, :], sin[:, None, :])
    kv = (rms_norm(c_kv, kv_norm_g) @ w_ukv).reshape(b, s, C_HEADS, C_NOPE_DIM + C_V_DIM)
    k_nope, v = kv[..., :C_NOPE_DIM], kv[..., C_NOPE_DIM:]
    k_r = apply_rope(k_rope, cos, sin)
    scale = (C_NOPE_DIM + C_ROPE_DIM) ** -0.5
    nq = s // C_Q_BLOCK
    qn_b = q_nope.reshape(b, nq, C_Q_BLOCK, C_HEADS, C_NOPE_DIM).swapaxes(0, 1)
    qr_b = q_rope.reshape(b, nq, C_Q_BLOCK, C_HEADS, C_ROPE_DIM).swapaxes(0, 1)
    k_pos = jnp.arange(s)

    def attend(args):
        qn, qr, blk = args
        sc = (jnp.einsum('bqhd,bkhd->bhqk', qn, k_nope, preferred_element_type=F32)
              + jnp.einsum('bqhd,bkd->bhqk', qr, k_r, preferred_element_type=F32)) * scale
        q_pos = blk * C_Q_BLOCK + jnp.arange(C_Q_BLOCK)
        sc = jnp.where(q_pos[:, None] >= k_pos[None, :], sc, -jnp.inf)
        p = jax.nn.softmax(sc, axis=-1).astype(v.dtype)
        return jnp.einsum('bhqk,bkhd->bqhd', p, v)

    out = lax.map(attend, (qn_b, qr_b, jnp.arange(nq)))
    return out.swapaxes(0, 1).reshape(b, s, C_OUT)


def hybrid_mixer(x, w_in, conv_w, conv_b, m_gate_b, m_norm_g, sinks,
                 q_norm_g, w_uq, kv_norm_g, w_ukv, w_out, cos, sin):
    b, s, _ = x.shape
    proj = x @ w_in
    a_q, a_k, a_v, m_q, m_k, m_v, m_o, m_if, c_q, c_kv, c_kr = split_columns(proj)
    y_a = sliding_window_attention(a_q.reshape(b, s, SWA_HEADS, SWA_HEAD_DIM),
                                   a_k.reshape(b, s, SWA_KV_HEADS, SWA_HEAD_DIM),
                                   a_v.reshape(b, s, SWA_KV_HEADS, SWA_HEAD_DIM), sinks)
    qk = jax.nn.silu(causal_depthwise_conv(jnp.concatenate([m_q, m_k], -1), conv_w, conv_b))
    m_q, m_k = jnp.split(qk, 2, axis=-1)
    gates = m_if.astype(F32) + m_gate_b.astype(F32)
    h = mlstm(m_q.reshape(b, s, M_HEADS, M_QK_DIM), m_k.reshape(b, s, M_HEADS, M_QK_DIM),
              m_v.reshape(b, s, M_HEADS, M_V_DIM), gates[..., :M_HEADS], gates[..., M_HEADS:])
    h = jax.nn.sigmoid(m_o.astype(F32)).reshape(b, s, M_HEADS, M_V_DIM) * h
    y_b = head_layer_norm(h, m_norm_g).astype(x.dtype).reshape(b, s, B_V)
    y_c = mla(c_q, c_kv, c_kr, q_norm_g, w_uq, kv_norm_g, w_ukv, cos, sin)
    return jnp.concatenate([y_a, y_b, y_c], axis=-1) @ w_out


def moe_ffn(x, w_router, b_router, w_gate_up, b_gate_up, w_down, b_down):
    b, s, d = x.shape
    t = b * s
    xt = x.reshape(t, d)
    logits = (xt @ w_router).astype(F32) + b_router.astype(F32)
    top_logit, top_e = lax.top_k(logits, TOP_K)
    gate = jax.nn.softmax(top_logit, axis=-1)
    n_assign = t * TOP_K
    flat_e = top_e.reshape(-1)
    order = jnp.argsort(flat_e, stable=True)
    sorted_e = flat_e[order]
    sorted_tok = order // TOP_K
    sorted_gate = gate.reshape(-1)[order]
    counts = jnp.bincount(flat_e, length=N_EXPERTS)
    padded = (counts + MOE_BLOCK - 1) // MOE_BLOCK * MOE_BLOCK
    pad_end = jnp.cumsum(padded)
    pad_start = pad_end - padded
    grp_start = jnp.cumsum(counts) - counts
    dest = pad_start[sorted_e] + jnp.arange(n_assign) - grp_start[sorted_e]
    n_blocks = -(-n_assign // MOE_BLOCK) + N_EXPERTS
    rows = n_blocks * MOE_BLOCK
    row_tok = jnp.zeros((rows,), jnp.int32).at[dest].set(sorted_tok.astype(jnp.int32))
    block_e = jnp.minimum(jnp.searchsorted(pad_end, jnp.arange(n_blocks) * MOE_BLOCK, side='right'),
                          N_EXPERTS - 1)
    xb = xt[row_tok].reshape(n_blocks, MOE_BLOCK, d)

    def expert_block(args):
        xe, e = args
        hg = xe @ w_gate_up[e] + b_gate_up[e]
        g_, u_ = jnp.split(hg, 2, axis=-1)
        g_ = jnp.minimum(g_, SWIGLU_LIMIT)
        u_ = jnp.clip(u_, -SWIGLU_LIMIT, SWIGLU_LIMIT)
        act = (u_ + 1.0) * g_ * jax.nn.sigmoid(SWIGLU_ALPHA * g_)
        return act @ w_down[e] + b_down[e]

    yb = lax.map(expert_block, (xb, block_e)).reshape(rows, d)
    y_assign = yb[dest].astype(F32) * sorted_gate[:, None]
    out = jnp.zeros((t, d), F32).at[sorted_tok].add(y_assign)
    return out.astype(x.dtype).reshape(b, s, d)


def setup_inputs(seed: int = 0) -> dict:
    key = jax.random.key(seed)
    ks = jax.random.split(key, 24)
    L = DEPTH

    def normal(k, shape, scale):
        return jax.random.normal(k, shape, F32) * scale

    x = normal(ks[0], (BATCH, SEQ, D_MODEL), 1.0)
    w_in = normal(ks[1], (L, D_MODEL, N_IN), D_MODEL ** -0.5)
    conv_w = normal(ks[2], (L, M_CONV, 2 * B_QK), M_CONV ** -0.5)
    conv_b = normal(ks[3], (L, 2 * B_QK), 0.02)
    i_bias = -3.0 + normal(ks[4], (L, M_HEADS), 0.1)
    f_bias = jnp.linspace(3.0, 6.0, M_HEADS, dtype=F32)[None, :] + normal(ks[5], (L, M_HEADS), 0.1)
    m_gate_b = jnp.concatenate([i_bias, f_bias], axis=-1)
    m_norm_g = 1.0 + normal(ks[6], (L, B_V), 0.02)
    sinks = normal(ks[7], (L, SWA_HEADS), 0.5)
    q_norm_g = 1.0 + normal(ks[8], (L, C_Q_LORA), 0.02)
    w_uq = normal(ks[9], (L, C_Q_LORA, C_HEADS * (C_NOPE_DIM + C_ROPE_DIM)), C_Q_LORA ** -0.5)
    kv_norm_g = 1.0 + normal(ks[10], (L, C_KV_LORA), 0.02)
    w_ukv = normal(ks[11], (L, C_KV_LORA, C_HEADS * (C_NOPE_DIM + C_V_DIM)), C_KV_LORA ** -0.5)
    w_out = normal(ks[12], (L, D_MIX, D_MODEL), D_MIX ** -0.5 * DN_BETA)
    ln1_g = 1.0 + normal(ks[13], (L, D_MODEL), 0.02)
    ln1_b = normal(ks[14], (L, D_MODEL), 0.02)
    w_router = normal(ks[15], (L, D_MODEL, N_EXPERTS), D_MODEL ** -0.5)
    b_router = normal(ks[16], (L, N_EXPERTS), 0.01)
    w_gate_up = normal(ks[17], (L, N_EXPERTS, D_MODEL, 2 * D_EXPERT), D_MODEL ** -0.5)
    b_gate_up = normal(ks[18], (L, N_EXPERTS, 2 * D_EXPERT), 0.02)
    w_down = normal(ks[19], (L, N_EXPERTS, D_EXPERT, D_MODEL), D_EXPERT ** -0.5 * DN_BETA)
    b_down = normal(ks[20], (L, N_EXPERTS, D_MODEL), 0.02)
    ln2_g = 1.0 + normal(ks[21], (L, D_MODEL), 0.02)
    ln2_b = normal(ks[22], (L, D_MODEL), 0.02)
    return {"x": x, "w_in": w_in, "conv_w": conv_w, "conv_b": conv_b, "m_gate_b": m_gate_b,
            "m_norm_g": m_norm_g, "sinks": sinks, "q_norm_g": q_norm_g, "w_uq": w_uq,
            "kv_norm_g": kv_norm_g, "w_ukv": w_ukv, "w_out": w_out, "ln1_g": ln1_g, "ln1_b": ln1_b,
            "w_router": w_router, "b_router": b_router, "w_gate_up": w_gate_up, "b_gate_up": b_gate_up,
            "w_down": w_down, "b_down": b_down, "ln2_g": ln2_g, "ln2_b": ln2_b}


def reference(x, w_in, conv_w, conv_b, m_gate_b, m_norm_g, sinks, q_norm_g, w_uq, kv_norm_g, w_ukv,
              w_out, ln1_g, ln1_b, w_router, b_router, w_gate_up, b_gate_up, w_down, b_down, ln2_g, ln2_b):
    cos, sin = rope_tables(x.shape[1], C_ROPE_DIM)
    for l in range(DEPTH):
        mix = hybrid_mixer(x, w_in[l], conv_w[l], conv_b[l], m_gate_b[l], m_norm_g[l], sinks[l],
                           q_norm_g[l], w_uq[l], kv_norm_g[l], w_ukv[l], w_out[l], cos, sin)
        x = layer_norm(DN_ALPHA * x + mix, ln1_g[l], ln1_b[l])
        ffn = moe_ffn(x, w_router[l], b_router[l], w_gate_up[l], b_gate_up[l], w_down[l], b_down[l])
        x = layer_norm(DN_ALPHA * x + ffn, ln2_g[l], ln2_b[l])
    return x
```

```python
import ml_dtypes
import numpy as np
from contextlib import ExitStack
import concourse.bass as bass
import concourse.mybir as mybir
from concourse.bass_utils import run_bass_kernel_spmd

F32 = mybir.dt.float32
BF16 = mybir.dt.bfloat16
I32 = mybir.dt.int32
ALU = mybir.AluOpType
AF = mybir.ActivationFunctionType
AX = mybir.AxisListType

ROT = 3000


class Prog:
    ENGS = ("pe", "act", "dve", "pool", "sp")

    def __init__(self, nc):
        self.nc = nc
        self.es = ExitStack()
        self.ops = {e: [] for e in self.ENGS}
        self.nsem = 0
        self.cur_sem = {}
        self.dma_sem = {}
        self.last_write = {}
        self.readers = {}
        self.waited = {e: {} for e in self.ENGS}
        self.out_tokens = []
        self.n_tensors = 0
        self.cc_toks = []

    def sb(self, shape, dtype, name=None):
        self.n_tensors += 1
        name = name or f"sb{self.n_tensors}"
        return self.es.enter_context(self.nc.sbuf_tensor(name, list(shape), dtype))

    def ps(self, shape, dtype, name=None):
        self.n_tensors += 1
        name = name or f"ps{self.n_tensors}"
        return self.es.enter_context(self.nc.psum_tensor(name, list(shape), dtype))

    def _new_sem(self):
        self.nsem += 1
        return self.nsem - 1

    def _deps(self, eng, reads, writes):
        toks = []
        for k in reads:
            t = self.last_write.get(k)
            if t is not None:
                toks.append(t)
        for k in writes:
            t = self.last_write.get(k)
            if t is not None:
                toks.append(t)
            toks.extend(self.readers.get(k, ()))
        waits = {}
        w = self.waited[eng]
        for (sid, val, _e) in toks:
            if w.get(sid, 0) >= val:
                continue
            if waits.get(sid, 0) < val:
                waits[sid] = val
        for sid, val in waits.items():
            w[sid] = val
        return list(waits.items())

    def _commit(self, tok, reads, writes):
        for k in writes:
            self.last_write[k] = tok
            self.readers[k] = []
        for k in reads:
            if k in writes:
                continue
            self.readers.setdefault(k, []).append(tok)

    def op(self, eng, fn, reads=(), writes=()):
        waits = self._deps(eng, reads, writes)
        sid, cnt = self.cur_sem.get(eng, (None, ROT))
        if cnt >= ROT:
            sid, cnt = self._new_sem(), 0
        cnt += 1
        self.cur_sem[eng] = (sid, cnt)
        tok = (sid, cnt, eng)
        self.ops[eng].append((waits, fn, sid, 1))
        self._commit(tok, reads, writes)
        return tok

    def I(self, eng, method, reads=(), writes=(), **kw):
        def fn(e, method=method, kw=kw):
            return getattr(e, method)(**kw)
        return self.op(eng, fn, reads, writes)

    def MM(self, specs, reads=(), writes=(), method="matmul"):
        specs = list(specs)

        def fn(e, specs=specs, method=method):
            ins = None
            for kw in specs:
                ins = getattr(e, method)(**kw)
            return ins
        return self.op("pe", fn, reads, writes)

    def dma(self, eng, out, in_, reads=(), writes=(), semkey=None, is_output=False, **kw):
        waits = self._deps(eng, reads, writes)
        key = semkey if semkey is not None else (tuple(writes) + tuple(reads))[0]
        sid, cnt = self.dma_sem.get(key, (None, ROT))
        if cnt >= ROT:
            sid, cnt = self._new_sem(), 0
        cnt += 1
        self.dma_sem[key] = (sid, cnt)
        tok = (sid, cnt * 16, "dma")

        def fn(e, out=out, in_=in_, kw=kw):
            return e.dma_start(out=out, in_=in_, **kw)

        self.ops[eng].append((waits, fn, sid, 16))
        self._commit(tok, reads, writes)
        if is_output:
            self.out_tokens.append(tok)
        return tok

    def cc(self, kind, op, groups, ins, outs, reads=(), writes=()):
        waits = self._deps("pool", reads, writes)
        sid = self._new_sem()
        tok = (sid, 16, "cc")

        def fn(e, kind=kind, op=op, groups=groups, ins=ins, outs=outs):
            return e.collective_compute(kind, op, replica_groups=groups, ins=ins, outs=outs)
        self.ops["pool"].append((waits, fn, sid, 16))
        self._commit(tok, reads, writes)
        self.cc_toks.append((sid, 16))
        return tok

    def barrier(self):
        toks = [(sid, cnt) for (sid, cnt) in self.cur_sem.values()]
        toks += [(sid, cnt * 16) for (sid, cnt) in self.dma_sem.values()]
        toks += list(self.cc_toks)
        for eng in self.ENGS:
            w = self.waited[eng]
            waits = []
            for sid, val in toks:
                if w.get(sid, 0) < val:
                    w[sid] = val
                    waits.append((sid, val))
            if waits:
                self.ops[eng].append((waits, None, None, 0))

    def emit(self):
        nc = self.nc
        final = {}
        for (sid, val, _e) in self.out_tokens:
            final[sid] = max(final.get(sid, 0), val)
        sems = [self.es.enter_context(nc.semaphore(f"s{i}")) for i in range(self.nsem)]
        ops = self.ops

        def replay(eng_name, extra_final=None):
            def body(e):
                for (waits, fn, sid, inc) in ops[eng_name]:
                    for (ws, wv) in waits:
                        e.wait_ge(sems[ws], wv)
                    if fn is None:
                        continue
                    ins = fn(e)
                    ins.then_inc(sems[sid], inc)
                if extra_final:
                    for ws, wv in extra_final.items():
                        e.wait_ge(sems[ws], wv)
            return body

        with nc.Block() as block:
            block.tensor(replay("pe"))
            block.scalar(replay("act"))
            block.vector(replay("dve"))
            block.gpsimd(replay("pool"))
            block.sync(replay("sp", final))
        self.es.close()
        return nc


ALPHA = float(4 ** 0.25)
LN_EPS = 1e-5


def layer_norm_tile(P, h, outf, gt, bt, st, mv, rstd, tag, extra_reads=(), out_writes=()):
    nc = P.nc
    for c in range(4):
        P.op("dve", lambda e, c=c: e.bn_stats(out=st[:, c * 6:(c + 1) * 6], in_=h[:, c * 512:(c + 1) * 512]),
             reads=[("h", tag)], writes=[("st", tag, c)])
    P.op("dve", lambda e: e.bn_aggr(out=mv[:, :], in_=st[:, :]),
         reads=[("st", tag, c) for c in range(4)], writes=[("mv", tag)])
    P.op("act", lambda e: e.activation(out=rstd[:, :], in_=mv[:, 1:2], func=AF.Sqrt, bias=LN_EPS, scale=1.0),
         reads=[("mv", tag)], writes=[("rs", tag)])
    P.op("dve", lambda e: e.reciprocal(out=rstd[:, :], in_=rstd[:, :]),
         reads=[("rs", tag)], writes=[("rs", tag)])
    P.op("dve", lambda e: e.tensor_scalar(out=h[:, :], in0=h[:, :], scalar1=mv[:, 0:1], scalar2=rstd[:, 0:1],
                                          op0=ALU.subtract, op1=ALU.mult),
         reads=[("mv", tag), ("rs", tag)], writes=[("h", tag)])
    P.op("pool", lambda e: e.tensor_tensor(out=h[:, :], in0=h[:, :], in1=gt[:, :], op=ALU.mult),
         reads=["lng"], writes=[("h", tag)])
    P.op("pool", lambda e: e.tensor_tensor(out=outf[:, :], in0=h[:, :], in1=bt[:, :], op=ALU.add),
         reads=["lnb", ("h", tag)] + list(extra_reads), writes=list(out_writes))


def build_O(nc):
    P = Prog(nc)
    D = lambda n, s, dt, kind="ExternalInput": nc.dram_tensor(n, s, dt, kind=kind).ap()
    yT = D("yT", [2048, 1024], F32)
    xres = D("xres", [1024, 2048], F32)
    wout = D("wout", [2048, 2048], F32)
    g1 = D("g1", [128, 2048], F32)
    b1 = D("b1", [128, 2048], F32)
    wr = D("wr", [2048, 32], F32)
    br = D("br", [128, 32], F32)
    ident_d = D("ident", [128, 128], F32)
    x1o = D("x1", [1024, 2048], F32, "ExternalOutput")
    x1bo = D("x1b", [1024, 2048], BF16, "ExternalOutput")
    Go = D("G", [1024, 32], F32, "ExternalOutput")

    wo = P.sb([128, 16, 2048], BF16)
    yb = P.sb([128, 16, 1024], BF16)
    gt = P.sb([128, 2048], F32)
    bt = P.sb([128, 2048], F32)
    wrt = P.sb([128, 16, 32], F32)
    brt = P.sb([128, 32], F32)
    ident = P.sb([128, 128], F32)
    xr = [P.sb([128, 2048], F32) for _ in range(2)]
    h = [P.sb([128, 2048], F32) for _ in range(2)]
    x1f = [P.sb([128, 2048], F32) for _ in range(2)]
    x1h = [P.sb([128, 2048], BF16) for _ in range(2)]
    x1T = [P.sb([128, 16, 128], F32) for _ in range(2)]
    st = [P.sb([128, 24], F32) for _ in range(2)]
    mv = [P.sb([128, 2], F32) for _ in range(2)]
    rstd = [P.sb([128, 1], F32) for _ in range(2)]
    lg = [P.sb([128, 32], F32) for _ in range(2)]
    top8 = [P.sb([128, 8], F32) for _ in range(2)]
    negm = [P.sb([128, 1], F32) for _ in range(2)]
    msk = [P.sb([128, 32], F32) for _ in range(2)]
    ex = [P.sb([128, 32], F32) for _ in range(2)]
    ssum = [P.sb([128, 1], F32) for _ in range(2)]
    gout = [P.sb([128, 32], F32) for _ in range(2)]
    pm = [P.ps([128, 512], F32) for _ in range(2)]
    ptr = [P.ps([128, 512], F32) for _ in range(2)]
    plg = P.ps([128, 32], F32)

    woK = [("wo", q) for q in range(4)]
    ybK = [("yb", q) for q in range(4)]
    for q in range(4):
        P.dma("pool", wo[:, 4 * q:4 * q + 4, :],
              wout[q * 512:(q + 1) * 512, :].rearrange("(k p) n -> p k n", p=128), writes=[woK[q]])
        P.dma("pool", yb[:, 4 * q:4 * q + 4, :],
              yT[q * 512:(q + 1) * 512, :].rearrange("(k p) n -> p k n", p=128), writes=[ybK[q]])
    P.dma("sp", gt[:, :], g1[:, :], writes=["lng"])
    P.dma("sp", bt[:, :], b1[:, :], writes=["lnb"])
    P.dma("sp", wrt[:, :, :], wr.rearrange("(k p) n -> p k n", p=128), writes=["wr"])
    P.dma("sp", brt[:, :], br[:, :], writes=["br"])
    P.dma("sp", ident[:, :], ident_d[:, :], writes=["ident"])

    nb = 0
    for t in range(8):
        s = t % 2
        P.dma("sp", xr[s][:, :], xres[t * 128:(t + 1) * 128, :], writes=[("xr", s)])
        for n in range(4):
            b = nb % 2
            nb += 1

            def mm(e, t=t, n=n, b=b):
                for k in range(16):
                    ins = e.matmul(pm[b][:, :], lhsT=yb[:, k, t * 128:(t + 1) * 128],
                                   rhs=wo[:, k, n * 512:(n + 1) * 512], start=(k == 0), stop=(k == 15))
                return ins
            P.op("pe", mm, reads=woK + ybK, writes=[("pm", b)])
            P.op("dve", lambda e, s=s, n=n, b=b: e.scalar_tensor_tensor(
                out=h[s][:, n * 512:(n + 1) * 512], in0=xr[s][:, n * 512:(n + 1) * 512], scalar=ALPHA,
                in1=pm[b][:, :], op0=ALU.mult, op1=ALU.add),
                reads=[("pm", b), ("xr", s)], writes=[("h", s)])
        layer_norm_tile(P, h[s], x1f[s], gt, bt, st[s], mv[s], rstd[s], s, out_writes=[("x1f", s)])
        P.dma("sp", x1o[t * 128:(t + 1) * 128, :], x1f[s][:, :], reads=[("x1f", s)], semkey=("x1o", s),
              is_output=True)
        P.op("act", lambda e, s=s: e.activation(out=x1h[s][:, :], in_=x1f[s][:, :], func=AF.Copy),
             reads=[("x1f", s)], writes=[("x1h", s)])
        P.dma("sp", x1bo[t * 128:(t + 1) * 128, :], x1h[s][:, :], reads=[("x1h", s)], semkey=("x1bo", s),
              is_output=True)
        for q in range(4):
            b = q % 2

            def tr(e, s=s, q=q, b=b):
                for j in range(4):
                    k = q * 4 + j
                    ins = e.transpose(ptr[b][:, j * 128:(j + 1) * 128], x1f[s][:, k * 128:(k + 1) * 128],
                                      ident[:, :])
                return ins
            P.op("pe", tr, reads=[("x1f", s), "ident"], writes=[("ptr", b)])
            P.op("act", lambda e, s=s, q=q, b=b: e.activation(
                out=x1T[s][:, q * 4:(q + 1) * 4, :].rearrange("p a b -> p (a b)"), in_=ptr[b][:, :], func=AF.Copy),
                reads=[("ptr", b)], writes=[("x1T", s, q)])

        def rmm(e, s=s):
            for k in range(16):
                ins = e.matmul(plg[:, :], lhsT=x1T[s][:, k, :], rhs=wrt[:, k, :], start=(k == 0), stop=(k == 15))
            return ins
        P.op("pe", rmm, reads=[("x1T", s, q) for q in range(4)] + ["wr"], writes=["plg"])
        P.op("dve", lambda e, s=s: e.tensor_tensor(out=lg[s][:, :], in0=plg[:, :], in1=brt[:, :], op=ALU.add),
             reads=["plg", "br"], writes=[("lg", s)])
        P.op("dve", lambda e, s=s: e.max(out=top8[s][:, :], in_=lg[s][:, :]), reads=[("lg", s)], writes=[("top8", s)])
        P.op("dve", lambda e, s=s: e.tensor_scalar(out=msk[s][:, :], in0=lg[s][:, :], scalar1=top8[s][:, 3:4],
                                                   scalar2=None, op0=ALU.is_ge),
             reads=[("lg", s), ("top8", s)], writes=[("msk", s)])
        P.op("dve", lambda e, s=s: e.tensor_scalar(out=negm[s][:, :], in0=top8[s][:, 0:1], scalar1=-1.0,
                                                   scalar2=None, op0=ALU.mult),
             reads=[("top8", s)], writes=[("negm", s)])
        P.op("act", lambda e, s=s: e.activation(out=ex[s][:, :], in_=lg[s][:, :], func=AF.Exp,
                                                bias=negm[s][:, 0:1], scale=1.0),
             reads=[("lg", s), ("negm", s)], writes=[("ex", s)])
        P.op("dve", lambda e, s=s: e.tensor_tensor(out=ex[s][:, :], in0=ex[s][:, :], in1=msk[s][:, :], op=ALU.mult),
             reads=[("msk", s)], writes=[("ex", s)])
        P.op("dve", lambda e, s=s: e.reduce_sum(out=ssum[s][:, :], in_=ex[s][:, :], axis=AX.X),
             reads=[("ex", s)], writes=[("ssum", s)])
        P.op("dve", lambda e, s=s: e.reciprocal(out=ssum[s][:, :], in_=ssum[s][:, :]),
             reads=[("ssum", s)], writes=[("ssum", s)])
        P.op("dve", lambda e, s=s: e.tensor_scalar(out=gout[s][:, :], in0=ex[s][:, :], scalar1=ssum[s][:, 0:1],
                                                   scalar2=None, op0=ALU.mult),
             reads=[("ex", s), ("ssum", s)], writes=[("gout", s)])
        P.dma("sp", Go[t * 128:(t + 1) * 128, :], gout[s][:, :], reads=[("gout", s)], semkey=("Go", s),
              is_output=True)
    return P.emit()


def host_consts_O():
    return {"ident": np.eye(128, dtype=np.float32)}


def build_N(nc):
    P = Prog(nc)
    D = lambda n, s, dt, kind="ExternalInput": nc.dram_tensor(n, s, dt, kind=kind).ap()
    parts = D("parts", [8 * 1024, 2048], F32)
    x1 = D("x1", [1024, 2048], F32)
    g2 = D("g2", [128, 2048], F32)
    b2 = D("b2", [128, 2048], F32)
    x2 = D("x2", [1024, 2048], F32, "ExternalOutput")

    gt = P.sb([128, 2048], F32)
    bt = P.sb([128, 2048], F32)
    pt = [P.sb([128, 2048], F32) for _ in range(8)]
    xr = [P.sb([128, 2048], F32) for _ in range(2)]
    h = [P.sb([128, 2048], F32) for _ in range(2)]
    of = [P.sb([128, 2048], F32) for _ in range(2)]
    st = [P.sb([128, 24], F32) for _ in range(2)]
    mv = [P.sb([128, 2], F32) for _ in range(2)]
    rstd = [P.sb([128, 1], F32) for _ in range(2)]
    P.dma("sp", gt[:, :], g2[:, :], writes=["lng"])
    P.dma("sp", bt[:, :], b2[:, :], writes=["lnb"])
    for t in range(8):
        s = t % 2
        P.dma("sp", xr[s][:, :], x1[t * 128:(t + 1) * 128, :], writes=[("xr", s)])
        for j in range(8):
            P.dma("sp", pt[j][:, :], parts[j * 1024 + t * 128:j * 1024 + (t + 1) * 128, :], writes=[("pt", j)])
        for half, eng in ((0, "dve"), (1, "pool")):
            c0, c1 = half * 1024, (half + 1) * 1024
            if eng == "dve":
                P.op(eng, lambda e, s=s, c0=c0, c1=c1: e.scalar_tensor_tensor(
                    out=h[s][:, c0:c1], in0=xr[s][:, c0:c1], scalar=ALPHA, in1=pt[0][:, c0:c1], op0=ALU.mult,
                    op1=ALU.add), reads=[("xr", s), ("pt", 0)], writes=[("hh", s, half), ("h", s)])
            else:
                P.op(eng, lambda e, s=s, c0=c0, c1=c1: e.tensor_scalar(
                    out=h[s][:, c0:c1], in0=xr[s][:, c0:c1], scalar1=ALPHA, scalar2=None, op0=ALU.mult),
                    reads=[("xr", s)], writes=[("hh", s, half), ("h", s)])
                P.op(eng, lambda e, s=s, c0=c0, c1=c1: e.tensor_tensor(
                    out=h[s][:, c0:c1], in0=h[s][:, c0:c1], in1=pt[0][:, c0:c1], op=ALU.add),
                    reads=[("pt", 0)], writes=[("hh", s, half)])
            for j in range(1, 8):
                P.op(eng, lambda e, s=s, j=j, c0=c0, c1=c1: e.tensor_tensor(
                    out=h[s][:, c0:c1], in0=h[s][:, c0:c1], in1=pt[j][:, c0:c1], op=ALU.add),
                    reads=[("pt", j)], writes=[("hh", s, half)])
        P.op("dve", lambda e, s=s: e.tensor_copy(out=st[s][:, 0:1], in_=st[s][:, 0:1]),
             reads=[("hh", s, 0), ("hh", s, 1)], writes=[("h", s)])
        layer_norm_tile(P, h[s], of[s], gt, bt, st[s], mv[s], rstd[s], s, out_writes=[("of", s)])
        P.dma("sp", x2[t * 128:(t + 1) * 128, :], of[s][:, :], reads=[("of", s)], semkey=("of", s), is_output=True)
    return P.emit()


CAP = 256
NG = 8
SW_LIMIT = 7.0
SW_ALPHA = 1.702


def build_E(nc):
    P = Prog(nc)
    D = lambda n, s, dt, kind="ExternalInput": nc.dram_tensor(n, s, dt, kind=kind).ap()
    x1b = D("x1b", [8192, 2048], BF16)
    Gc = D("Gc", [8192, 4], F32)
    wgu = D("wgu", [4 * 2048, 4096], F32)
    bgut = D("bgut", [4 * 128, 32], F32)
    wdd = D("wd", [4 * 2048, 2048], F32)
    bdr = D("bdr", [4 * 128, 2048], F32)
    iota_d = D("iota_row", [128, CAP], F32)
    ustr_d = D("ustrict", [128, 128], F32)
    ident_d = D("ident", [128, 128], F32)
    part = D("part", [8192, 2048], F32, "ExternalOutput")
    ysc = D("ysc", [4 * 2048, 2048], BF16, "Internal")

    big = P.sb([128, 32768], BF16)
    xgT = big[:, 0:16384].rearrange("p (k s) -> p k s", k=16)
    actT = big[:, 16384:32768].rearrange("p (k s) -> p k s", k=16)
    ysb = [big[:, 0:16384].rearrange("p (a f) -> p a f", a=8), big[:, 16384:32768].rearrange("p (a f) -> p a f", a=8)]
    REG = ["RA", "RB"]
    xin = [P.sb([128, 8, 512], BF16) for _ in range(2)]
    wg = [P.sb([128, 16, 128], BF16) for _ in range(2)]
    wu = [P.sb([128, 16, 128], BF16) for _ in range(2)]
    wdc = [P.sb([128, 16, 512], BF16) for _ in range(2)]
    S = [P.sb([128, 8, CAP], BF16) for _ in range(2)]
    SGT = P.sb([128, 8, 1024], BF16)
    gq = [P.sb([128, 512], F32) for _ in range(2)]
    sg = [P.sb([128, 512], F32) for _ in range(2)]
    uq = [P.sb([128, 512], F32) for _ in range(2)]
    yst = [P.sb([128, 512], BF16) for _ in range(4)]
    otile = [P.sb([128, 2048], F32) for _ in range(2)]
    bg = P.sb([128, 32], F32)
    bdt = P.sb([128, 2048], F32)
    Gt = P.sb([128, 256], F32)
    maskf = P.sb([128, 256], F32)
    maskb = P.sb([128, 256], BF16)
    posm = P.sb([128, 256], F32)
    iota = P.sb([128, CAP], F32)
    ustr_f = P.sb([128, 128], F32)
    ident_f = P.sb([128, 128], F32)
    ustr = P.sb([128, 128], BF16)
    ones = P.sb([128, 128], BF16)
    ident = P.sb([128, 128], BF16)

    pA = [P.ps([128, 512], F32) for _ in range(2)]
    pH = [[P.ps([128, 512], F32) for _ in range(2)] for _ in range(2)]
    pM = P.ps([128, 256], F32)
    pT = P.ps([128, 1024], BF16)

    P.dma("sp", iota[:, :], iota_d[:, :], writes=["iota"])
    P.dma("sp", ustr_f[:, :], ustr_d[:, :], writes=["ustr_f"])
    P.dma("sp", ident_f[:, :], ident_d[:, :], writes=["ident_f"])
    P.dma("sp", Gt[:, :].rearrange("p (n e) -> p n e", e=4), Gc.rearrange("(n p) e -> p n e", p=128), writes=["Gt"])
    P.op("dve", lambda e: e.tensor_copy(out=ustr[:, :], in_=ustr_f[:, :]), reads=["ustr_f"], writes=["ustr"])
    P.op("dve", lambda e: e.tensor_copy(out=ident[:, :], in_=ident_f[:, :]), reads=["ident_f"], writes=["ident"])
    P.op("dve", lambda e: e.memset(ones[:, :], 1.0), writes=["ones"])
    P.op("dve", lambda e: e.tensor_scalar(out=maskf[:, :], in0=Gt[:, :], scalar1=0.0, scalar2=None, op0=ALU.is_gt),
         reads=["Gt"], writes=["maskf"])
    P.op("dve", lambda e: e.tensor_copy(out=maskb[:, :], in_=maskf[:, :]), reads=["maskf"], writes=["maskb"])

    def posmm(e):
        ins = None
        for g in range(NG):
            for i in range(8):
                n = g * 8 + i
                for i2 in range(i):
                    n2 = g * 8 + i2
                    e.matmul(pM[:, n * 4:(n + 1) * 4], lhsT=ones[:, :], rhs=maskb[:, n2 * 4:(n2 + 1) * 4],
                             start=(i2 == 0), stop=False)
                ins = e.matmul(pM[:, n * 4:(n + 1) * 4], lhsT=ustr[:, :], rhs=maskb[:, n * 4:(n + 1) * 4],
                               start=(i == 0), stop=True)
        return ins
    P.op("pe", posmm, reads=["ones", "ustr", "maskb"], writes=["pM"])
    P.op("dve", lambda e: e.scalar_tensor_tensor(out=posm[:, :], in0=pM[:, :], scalar=1.0, in1=maskf[:, :],
                                                 op0=ALU.add, op1=ALU.mult),
         reads=["pM", "maskf"], writes=["posm"])
    P.op("dve", lambda e: e.tensor_scalar(out=posm[:, :], in0=posm[:, :], scalar1=-1.0, scalar2=None, op0=ALU.add),
         reads=["posm"], writes=["posm"])

    items = []
    cnt = {"xin": 0, "w": 0, "wd": 0, "pA": 0, "pH": 0, "t": 0, "yst": 0, "cp": 0}

    def nxt(k, mod):
        v = cnt[k] % mod
        cnt[k] += 1
        return v

    for ex in range(4):
        def ld_bias(ex=ex):
            P.dma("sp", bg[:, :], bgut[ex * 128:(ex + 1) * 128, :], writes=["bg"])
            P.dma("sp", bdt[:, :], bdr[ex * 128:(ex + 1) * 128, :], writes=["bdt"])
            P.op("dve", lambda e: e.tensor_scalar(out=bg[:, 16:32], in0=bg[:, 16:32], scalar1=1.0, scalar2=None,
                                                  op0=ALU.add), reads=["bg"], writes=["bg"])
        items.append((None, ld_bias))
        for hf in range(2):
            for gg in range(4):
                g = hf * 4 + gg
                sbuf_i = g % 2
                for fq in range(4):
                    xb = nxt("xin", 2)

                    def ld(g=g, fq=fq, xb=xb):
                        P.dma("sp", xin[xb][:, :, :],
                              x1b[g * 1024:(g + 1) * 1024, fq * 512:(fq + 1) * 512].rearrange("(n p) f -> p n f", p=128),
                              writes=[("xin", xb)])

                    def cp(g=g, gg=gg, fq=fq, xb=xb, ex=ex, sbuf_i=sbuf_i):
                        if fq == 0:
                            for i in range(8):
                                n = g * 8 + i
                                P.op("dve", lambda e, i=i, n=n: e.tensor_scalar(
                                    out=S[sbuf_i][:, i, :], in0=iota[:, :], scalar1=posm[:, n * 4 + ex:n * 4 + ex + 1],
                                    scalar2=None, op0=ALU.is_equal),
                                    reads=["iota", "posm"], writes=[("S", sbuf_i, i)])
                        for kp in range(2):
                            b = nxt("pA", 2)

                            def mm(e, kp=kp, b=b):
                                for kk in range(2):
                                    kl = kp * 2 + kk
                                    for i in range(8):
                                        ins = e.matmul(pA[b][:, kk * CAP:(kk + 1) * CAP],
                                                       lhsT=xin[xb][:, i, kl * 128:(kl + 1) * 128],
                                                       rhs=S[sbuf_i][:, i, :], start=(i == 0), stop=(i == 7))
                                return ins
                            P.op("pe", mm, reads=[("xin", xb)] + [("S", sbuf_i, i) for i in range(8)],
                                 writes=[("pA", b)])
                            k0 = fq * 4 + kp * 2
                            dst = xgT[:, k0:k0 + 2, gg * CAP:(gg + 1) * CAP]
                            src = pA[b][:, :].rearrange("p (a c) -> p a c", a=2)
                            if nxt("cp", 2) == 0:
                                P.op("act", lambda e, dst=dst, src=src: e.activation(out=dst, in_=src, func=AF.Copy),
                                     reads=[("pA", b), "RA"], writes=[("xgT", k0, gg), ("xgT", k0 + 1, gg)])
                            else:
                                P.op("dve", lambda e, dst=dst, src=src: e.tensor_copy(out=dst, in_=src),
                                     reads=[("pA", b), "RA"], writes=[("xgT", k0, gg), ("xgT", k0 + 1, gg)])
                    items.append((ld, cp))
            for m in range(16):
                wb = nxt("w", 2)

                def ld(ex=ex, m=m, wb=wb):
                    P.dma("pool", wg[wb][:, :, :],
                          wgu[ex * 2048:(ex + 1) * 2048, m * 128:(m + 1) * 128].rearrange("(k p) n -> p k n", p=128),
                          writes=[("wg", wb)])
                    P.dma("pool", wu[wb][:, :, :],
                          wgu[ex * 2048:(ex + 1) * 2048, 2048 + m * 128:2048 + (m + 1) * 128].rearrange(
                              "(k p) n -> p k n", p=128),
                          writes=[("wu", wb)])

                def cp(m=m, wb=wb):
                    for q in range(2):
                        hb = nxt("pH", 2)
                        tb = nxt("t", 2)
                        xk = [("xgT", k, gg) for k in range(16) for gg in (2 * q, 2 * q + 1)]

                        def mm(e, q=q, hb=hb):
                            for k in range(16):
                                e.matmul(pH[hb][0][:, :], lhsT=wg[wb][:, k, :], rhs=xgT[:, k, q * 512:(q + 1) * 512],
                                         start=(k == 0), stop=(k == 15))
                            for k in range(16):
                                ins = e.matmul(pH[hb][1][:, :], lhsT=wu[wb][:, k, :],
                                               rhs=xgT[:, k, q * 512:(q + 1) * 512], start=(k == 0), stop=(k == 15))
                            return ins
                        P.op("pe", mm, reads=[("wg", wb), ("wu", wb), "RA"] + xk, writes=[("pH", hb)])
                        P.op("dve", lambda e, hb=hb, tb=tb: e.tensor_scalar(
                            out=gq[tb][:, :], in0=pH[hb][0][:, :], scalar1=bg[:, m:m + 1], scalar2=SW_LIMIT,
                            op0=ALU.add, op1=ALU.min), reads=[("pH", hb), "bg"], writes=[("gq", tb)])
                        P.op("act", lambda e, tb=tb: e.activation(out=sg[tb][:, :], in_=gq[tb][:, :], func=AF.Sigmoid,
                                                                  scale=SW_ALPHA),
                             reads=[("gq", tb)], writes=[("sg", tb)])
                        P.op("dve", lambda e, hb=hb, tb=tb: e.tensor_scalar(
                            out=uq[tb][:, :], in0=pH[hb][1][:, :], scalar1=bg[:, 16 + m:17 + m], scalar2=1.0 - SW_LIMIT,
                            op0=ALU.add, op1=ALU.max), reads=[("pH", hb), "bg"], writes=[("uq", tb)])
                        P.op("dve", lambda e, tb=tb: e.tensor_tensor(out=gq[tb][:, :], in0=gq[tb][:, :],
                                                                     in1=sg[tb][:, :], op=ALU.mult),
                             reads=[("sg", tb)], writes=[("gq", tb)])
                        P.op("dve", lambda e, tb=tb, q=q: e.scalar_tensor_tensor(
                            out=actT[:, m, q * 512:(q + 1) * 512], in0=uq[tb][:, :], scalar=1.0 + SW_LIMIT,
                            in1=gq[tb][:, :], op0=ALU.min, op1=ALU.mult),
                            reads=[("uq", tb), ("gq", tb), "RB"], writes=[("actT", m, q)])
                items.append((ld, cp))
            for n in range(4):
                db = nxt("wd", 2)

                def ld(ex=ex, n=n, db=db):
                    P.dma("pool", wdc[db][:, :, :],
                          wdd[ex * 2048:(ex + 1) * 2048, n * 512:(n + 1) * 512].rearrange("(m p) c -> p m c", p=128),
                          writes=[("wdc", db)])

                def cp(ex=ex, hf=hf, n=n, db=db):
                    for s in range(8):
                        b = nxt("pA", 2)
                        yb_ = nxt("yst", 4)

                        def mm(e, s=s, b=b):
                            for m in range(16):
                                ins = e.matmul(pA[b][:, :], lhsT=actT[:, m, s * 128:(s + 1) * 128], rhs=wdc[db][:, m, :],
                                               start=(m == 0), stop=(m == 15))
                            return ins
                        P.op("pe", mm, reads=[("wdc", db), "RB"] + [("actT", m, s // 4) for m in range(16)],
                             writes=[("pA", b)])
                        P.op("dve", lambda e, b=b, yb_=yb_: e.tensor_tensor(
                            out=yst[yb_][:, :], in0=pA[b][:, :], in1=bdt[:, n * 512:(n + 1) * 512], op=ALU.add),
                            reads=[("pA", b), "bdt"], writes=[("yst", yb_)])
                        r0 = ex * 2048 + hf * 1024 + s * 128
                        P.dma("sp", ysc[r0:r0 + 128, n * 512:(n + 1) * 512], yst[yb_][:, :],
                              reads=[("yst", yb_)], writes=[("ysc", ex, hf, s, n)], semkey=("yst", yb_))
                items.append((ld, cp))

    for idx, (ld, cp) in enumerate(items):
        if idx == 0 and ld is not None:
            ld()
        if idx + 1 < len(items) and items[idx + 1][0] is not None:
            items[idx + 1][0]()
        cp()

    for g in range(NG):
        hf, gg = g // 4, g % 4
        yb_ = g % 2
        for ex in range(4):
            r0 = ex * 2048 + g * CAP
            P.dma("sp", ysb[yb_][:, 2 * ex:2 * ex + 2, :], ysc[r0:r0 + CAP, :].rearrange("(s p) f -> p s f", p=128),
                  reads=[("ysc", ex, hf, gg * 2 + s, n) for s in range(2) for n in range(4)],
                  writes=[REG[yb_], ("ysb", yb_, ex)], semkey=("ysb", yb_, ex))
        for ex in range(4):
            sbuf_i = ex % 2
            for i in range(8):
                n = g * 8 + i
                P.op("dve", lambda e, i=i, n=n, ex=ex, sbuf_i=sbuf_i: e.tensor_scalar(
                    out=S[sbuf_i][:, i, :], in0=iota[:, :], scalar1=posm[:, n * 4 + ex:n * 4 + ex + 1],
                    scalar2=Gt[:, n * 4 + ex:n * 4 + ex + 1], op0=ALU.is_equal, op1=ALU.mult),
                    reads=["iota", "posm", "Gt"], writes=[("S", sbuf_i, i)])
            for s in range(2):
                def tr(e, s=s, sbuf_i=sbuf_i):
                    for i in range(8):
                        ins = e.transpose(pT[:, i * 128:(i + 1) * 128], S[sbuf_i][:, i, s * 128:(s + 1) * 128],
                                          ident[:, :])
                    return ins
                P.op("pe", tr, reads=["ident"] + [("S", sbuf_i, i) for i in range(8)], writes=["pT"])
                if s == 0:
                    P.op("act", lambda e, ex=ex, s=s: e.activation(out=SGT[:, 2 * ex + s, :], in_=pT[:, :], func=AF.Copy),
                         reads=["pT"], writes=[("SGT", 2 * ex + s)])
                else:
                    P.op("dve", lambda e, ex=ex, s=s: e.tensor_copy(out=SGT[:, 2 * ex + s, :], in_=pT[:, :]),
                         reads=["pT"], writes=[("SGT", 2 * ex + s)])
        for i in range(8):
            ob = (g * 8 + i) % 2
            for n in range(4):
                hb = nxt("pH", 2)

                def mm(e, i=i, n=n, hb=hb, yb_=yb_):
                    for es in range(8):
                        ins = e.matmul(pH[hb][0][:, :], lhsT=SGT[:, es, i * 128:(i + 1) * 128],
                                       rhs=ysb[yb_][:, es, n * 512:(n + 1) * 512], start=(es == 0), stop=(es == 7))
                    return ins
                P.op("pe", mm, reads=[("SGT", es) for es in range(8)] + [("ysb", yb_, ex) for ex in range(4)],
                     writes=[("pH", hb)])
                if n % 2 == 0:
                    P.op("act", lambda e, ob=ob, n=n, hb=hb: e.activation(
                        out=otile[ob][:, n * 512:(n + 1) * 512], in_=pH[hb][0][:, :], func=AF.Copy),
                        reads=[("pH", hb)], writes=[("otile", ob, n)])
                else:
                    P.op("dve", lambda e, ob=ob, n=n, hb=hb: e.tensor_copy(
                        out=otile[ob][:, n * 512:(n + 1) * 512], in_=pH[hb][0][:, :]),
                        reads=[("pH", hb)], writes=[("otile", ob, n)])
            t0 = g * 1024 + i * 128
            P.dma("sp", part[t0:t0 + 128, :], otile[ob][:, :], reads=[("otile", ob, n) for n in range(4)],
                  writes=[("otile_dma", ob)], semkey=("otile", ob), is_output=True)
    return P.emit()


def host_consts_E():
    return {"iota_row": np.tile(np.arange(CAP, dtype=np.float32), (128, 1)),
            "ustrict": np.triu(np.ones((128, 128), np.float32), 1),
            "ident": np.eye(128, dtype=np.float32)}


RMS_EPS = 1e-6
LNH_EPS = 1e-5
NT = 8
SC_A = 64 ** -0.5
SC_C = 192 ** -0.5
SC_B = 64 ** -0.5


def build_M(nc, do_A=True, do_B=True, do_C=True):
    P = Prog(nc)
    D = lambda n, s, dt, kind="ExternalInput": nc.dram_tensor(n, s, dt, kind=kind).ap()
    xT = D("xT", [2048, 4096], F32)
    wA = D("wA", [2048, 256], F32)
    wB = D("wB", [2048, 386], F32)
    wC = D("wC", [2048, 896], F32)
    wuq = D("wuq", [512, 512], F32)
    gq_col = D("gq_col", [128, 4], F32)
    wukv = D("wukv", [256, 512], F32)
    gkv_col = D("gkv_col", [128, 2], F32)
    cw_d = D("cw", [64, 8], F32)
    cb_d = D("cb", [64, 2], F32)
    gateb_d = D("gateb", [1, 2], F32)
    mng_d = D("mng", [128, 1], F32)
    sinks_d = D("sinks", [64, 2], F32)
    cosT = D("cosT", [64, 4096], F32)
    sinT = D("sinT", [64, 4096], F32)
    maskA_d = D("maskA", [5 * 128, 512], BF16)
    maskC_d = D("maskC", [4 * 128, 512], BF16)
    yT = D("yT", [512, 4096], F32, "ExternalOutput")

    B16 = [P.sb([128, 4096], BF16) for _ in range(5)]
    xTb = [P.sb([128, 16, 512], BF16) for _ in range(2)]
    Wa = P.sb([128, 16, 896], BF16)
    ones_b = P.sb([128, 128], BF16)
    ones_f = P.sb([128, 128], F32)
    onesdiv_f = P.sb([128, 128], F32)
    mskA = P.sb([128, 5, 512], BF16)
    mskC = P.sb([128, 4, 512], BF16)
    PT = [P.sb([128, 512], BF16) for _ in range(3)]
    Wt = [P.sb([128, 512], F32) for _ in range(2)]
    fA = [P.sb([128, 512], F32) for _ in range(2)]
    fB = [P.sb([128, 512], F32) for _ in range(2)]
    fC = [P.sb([128, 512], F32) for _ in range(2)]
    oT = [P.sb([128, 512], F32) for _ in range(2)]
    small = P.sb([128, 64], F32)
    pS = [P.ps([128, 512], F32) for _ in range(2)]
    pO = [P.ps([128, 512], F32) for _ in range(2)]
    pD = P.ps([128, 512], F32)
    pX = [P.ps([128, 512], F32) for _ in range(2)]
    pcol = P.ps([128, 8], F32)

    cnt = {}

    def nxt(k, mod):
        v = cnt.get(k, 0)
        cnt[k] = v + 1
        return v % mod

    P.I("dve", "memset", writes=["ones_b"], ap=ones_b[:, :], constant=1.0)
    P.I("dve", "memset", writes=["ones_f"], ap=ones_f[:, :], constant=1.0)
    P.I("dve", "memset", writes=["onesdiv_f"], ap=onesdiv_f[:, :], constant=1.0 / 128.0)
    P.dma("sp", mskA[:, :, :], maskA_d.rearrange("(r p) q -> p r q", p=128), writes=["mskA"])
    P.dma("sp", mskC[:, :, :], maskC_d.rearrange("(r p) q -> p r q", p=128), writes=["mskC"])

    def load_x(tt):
        xb = nxt("xTb", 2)
        P.dma("pool", xTb[xb][:, :, :], xT[:, tt * 512:(tt + 1) * 512].rearrange("(k p) t -> p k t", p=128),
              writes=[("xTb", xb)])
        return xb

    def proj_fm(xb, lo, hi, M):
        b = nxt("pX", 2)
        P.MM([dict(out=pX[b][0:M, :], lhsT=Wa[:, k, lo:hi], rhs=xTb[xb][:, k, :], start=(k == 0), stop=(k == 15))
              for k in range(16)], reads=["Wa", ("xTb", xb)], writes=[("pX", b)])
        return b

    def proj_tm(xb, sub, lo, hi):
        b = nxt("pX", 2)
        P.MM([dict(out=pX[b][:, 0:hi - lo], lhsT=xTb[xb][:, k, sub * 128:(sub + 1) * 128], rhs=Wa[:, k, lo:hi],
                   start=(k == 0), stop=(k == 15)) for k in range(16)],
             reads=["Wa", ("xTb", xb)], writes=[("pX", b)])
        return b

    def copy_out(eng, dst, src, reads, writes):
        if eng == "act":
            P.I("act", "activation", reads=reads, writes=writes, out=dst, in_=src, func=AF.Copy)
        else:
            P.I(eng, "tensor_copy", reads=reads, writes=writes, out=dst, in_=src)

    def attention(Slist, score_specs, score_reads, prob_fn, v_ap, vkey, M, finish_fn):
        n = len(Slist)
        pob = nxt("pO", 2)
        po = pO[pob]

        def pv(idx, S, ptb):
            P.MM([dict(out=po[0:M, :], lhsT=v_ap(S), rhs=PT[ptb][:, :], start=(idx == 0), stop=(idx == n - 1)),
                  dict(out=pD[0:M, :], lhsT=ones_b[:, 0:M], rhs=PT[ptb][:, :], start=(idx == 0), stop=(idx == n - 1))],
                 reads=[("PT", ptb), "ones_b", (vkey, S)], writes=[("pO", pob), "pD"])
        pend = None
        for idx, S in enumerate(Slist):
            psb = nxt("pS", 2)
            ptb = nxt("PT", 3)
            P.MM(score_specs(pS[psb], S), reads=score_reads(S), writes=[("pS", psb)])
            prob_fn(S, psb, ptb)
            if pend is not None:
                pv(*pend)
            pend = (idx, S, ptb)
        pv(*pend)
        finish_fn(po, ("pO", pob))

    if do_A:
        aq = [B16[0], B16[1]]
        ak = B16[2]
        aV = B16[3][:, 0:2048].rearrange("p (n d) -> p n d", d=64)
        P.dma("pool", Wa[:, :, 0:256], wA.rearrange("(k p) n -> p k n", p=128), writes=["Wa"])
        P.dma("sp", small[0:64, 0:2], sinks_d[:, :], writes=["sinks"])
        P.I("act", "activation", reads=["sinks"], writes=["esink"], out=small[0:64, 2:4], in_=small[0:64, 0:2],
            func=AF.Exp)
        for tt in range(NT):
            xb = load_x(tt)
            for hq in range(2):
                b = proj_fm(xb, hq * 64, hq * 64 + 64, 64)
                copy_out("act", aq[hq][0:64, tt * 512:(tt + 1) * 512], pX[b][0:64, :], [("pX", b)], [("aq", hq, tt)])
            b = proj_fm(xb, 128, 192, 64)
            copy_out("dve", ak[0:64, tt * 512:(tt + 1) * 512], pX[b][0:64, :], [("pX", b)],
                     [("ak", tt * 4 + c) for c in range(4)])
            for sub in range(4):
                b = proj_tm(xb, sub, 192, 256)
                copy_out("dve" if sub % 2 else "act", aV[:, tt * 4 + sub, :], pX[b][:, 0:64], [("pX", b)],
                         [("aV", tt * 4 + sub)])

        def swa_tile(hq, T):
            Slist = [4 * T - 1 + r for r in range(5) if 4 * T - 1 + r >= 0]

            def score_specs(ps, S):
                return [dict(out=ps[:, :], lhsT=ak[0:64, S * 128:(S + 1) * 128],
                             rhs=aq[hq][0:64, T * 512:(T + 1) * 512], start=True, stop=True)]

            def prob_fn(S, psb, ptb):
                r = S - (4 * T - 1)
                P.I("act", "activation", reads=[("pS", psb)], writes=[("PT", ptb)], out=PT[ptb][:, :],
                    in_=pS[psb][:, :], func=AF.Exp, scale=SC_A)
                P.I("pool", "tensor_tensor", reads=["mskA"], writes=[("PT", ptb)], out=PT[ptb][:, :],
                    in0=PT[ptb][:, :], in1=mskA[:, r, :], op=ALU.mult)

            def finish(po, pokey):
                ob = nxt("oT", 2)
                P.I("dve", "tensor_scalar", reads=["pD", "esink"], writes=[("fA", ob)], out=fA[ob][0:64, :],
                    in0=pD[0:64, :], scalar1=small[0:64, 2 + hq:3 + hq], scalar2=None, op0=ALU.add)
                P.I("dve", "reciprocal", reads=[("fA", ob)], writes=[("fA", ob)], out=fA[ob][0:64, :],
                    in_=fA[ob][0:64, :])
                P.I("dve", "tensor_tensor", reads=[pokey, ("fA", ob)], writes=[("oT", ob)], out=oT[ob][0:64, :],
                    in0=po[0:64, :], in1=fA[ob][0:64, :], op=ALU.mult)
                P.dma("sp", yT[hq * 64:(hq + 1) * 64, T * 512:(T + 1) * 512], oT[ob][0:64, :],
                      reads=[("oT", ob)], semkey=("oT", ob), is_output=True)
            attention(Slist, score_specs, lambda S: [("ak", S), ("aq", hq, T)], prob_fn,
                      lambda S: aV[:, S, :], "aV", 64, finish)

        for hq in range(2):
            for T in range(NT):
                swa_tile(hq, T)

    if do_C:
        P.barrier()
        wst = P.sb([128, 4, 256], F32)
        wqn = P.sb([128, 4, 128], BF16)
        wqr = P.sb([128, 4, 64], BF16)
        wqrs = P.sb([128, 4, 64], BF16)
        wkn = P.sb([128, 2, 128], BF16)
        wv = P.sb([128, 2, 128], BF16)
        cqb = [P.sb([128, 4, 512], BF16) for _ in range(2)]
        sqb = [P.sb([128, 4, 512], BF16) for _ in range(2)]
        ckvb = [P.sb([128, 2, 512], BF16) for _ in range(2)]
        sqkvb = [P.sb([128, 2, 512], BF16) for _ in range(2)]
        cst1 = P.sb([64, 512], F32)
        cst = [cst1, cst1]
        snt1 = P.sb([64, 512], F32)
        snt = [snt1, snt1]
        P.dma("pool", Wa[:, :, :], wC.rearrange("(k p) n -> p k n", p=128), writes=["Wa"])
        P.dma("sp", small[:, 8:12], gq_col[:, :], writes=["gq_col"])
        P.dma("sp", small[:, 12:14], gkv_col[:, :], writes=["gkv_col"])
        qn, qr, kn, kr = B16[0], B16[1], B16[2], B16[3]
        Vh = B16[4].rearrange("p (n d) -> p n d", d=128)

        def mla_weights(hh):
            P.dma("sp", wst[:, :, :], wuq[:, hh * 256:(hh + 1) * 256].rearrange("(c p) n -> p c n", p=128),
                  writes=["wst"])
            for c in range(4):
                g = small[:, 8 + c:9 + c]
                P.I("dve", "tensor_scalar", reads=["wst", "gq_col"], writes=["wqn"], out=wqn[:, c, :],
                    in0=wst[:, c, 0:128], scalar1=g, scalar2=None, op0=ALU.mult)
                P.I("dve", "tensor_scalar", reads=["wst", "gq_col"], writes=["wqr"], out=wqr[:, c, :],
                    in0=wst[:, c, 128:192], scalar1=g, scalar2=None, op0=ALU.mult)
                P.I("dve", "tensor_scalar", reads=["wst", "gq_col"], writes=["wqrs"], out=wqrs[:, c, :],
                    in0=wst[:, c, 192:256], scalar1=g, scalar2=None, op0=ALU.mult)
            P.dma("sp", wst[:, 0:2, :], wukv[:, hh * 256:(hh + 1) * 256].rearrange("(c p) n -> p c n", p=128),
                  writes=["wst"])
            for c in range(2):
                g = small[:, 12 + c:13 + c]
                P.I("dve", "tensor_scalar", reads=["wst", "gkv_col"], writes=["wkn"], out=wkn[:, c, :],
                    in0=wst[:, c, 0:128], scalar1=g, scalar2=None, op0=ALU.mult)
                P.I("dve", "tensor_scalar", reads=["wst", "gkv_col"], writes=["wv"], out=wv[:, c, :],
                    in0=wst[:, c, 128:256], scalar1=g, scalar2=None, op0=ALU.mult)

        def rope(specs_a, specs_b, reads, tb, dst, dkeys, scale_key=None, scale_t=None):
            ba = nxt("pX", 2)
            P.MM(specs_a(pX[ba]), reads=reads, writes=[("pX", ba)])
            ra = nxt("fC", 2)
            P.I("dve", "tensor_tensor", reads=[("pX", ba), ("cst", 0)], writes=[("fC", ra)], out=fC[ra][0:64, :],
                in0=pX[ba][0:64, :], in1=cst[tb][:, :], op=ALU.mult)
            bb = nxt("pX", 2)
            P.MM(specs_b(pX[bb]), reads=reads, writes=[("pX", bb)])
            rb = nxt("fC", 2)
            P.I("dve", "tensor_tensor", reads=[("pX", bb), ("snt", 0)], writes=[("fC", rb)], out=fC[rb][0:64, :],
                in0=pX[bb][0:64, :], in1=snt[tb][:, :], op=ALU.mult)
            if scale_key is None:
                P.I("pool", "tensor_tensor", reads=[("fC", ra), ("fC", rb)], writes=dkeys, out=dst,
                    in0=fC[ra][0:64, :], in1=fC[rb][0:64, :], op=ALU.add)
            else:
                P.I("pool", "tensor_tensor", reads=[("fC", rb)], writes=[("fC", ra)], out=fC[ra][0:64, :],
                    in0=fC[ra][0:64, :], in1=fC[rb][0:64, :], op=ALU.add)
                P.I("pool", "tensor_tensor", reads=[("fC", ra), scale_key], writes=dkeys, out=dst,
                    in0=fC[ra][0:64, :], in1=scale_t[0:64, :], op=ALU.mult)

        def mla_proj_tile(tt):
            xb = load_x(tt)
            tb = tt % 2
            tsl = slice(tt * 512, (tt + 1) * 512)
            P.dma("sp", cst[tb][:, :], cosT[:, tsl], writes=[("cst", 0)])
            P.dma("sp", snt[tb][:, :], sinT[:, tsl], writes=[("snt", 0)])
            for c in range(4):
                b = proj_fm(xb, c * 128, (c + 1) * 128, 128)
                P.I("act", "activation", reads=[("pX", b)], writes=[("cqb", tb, c)], out=cqb[tb][:, c, :],
                    in_=pX[b][:, :], func=AF.Copy)
                P.I("act", "activation", reads=[("pX", b)], writes=[("sqb", tb, c)], out=sqb[tb][:, c, :],
                    in_=pX[b][:, :], func=AF.Square)
            for c in range(2):
                b = proj_fm(xb, 512 + c * 128, 512 + (c + 1) * 128, 128)
                P.I("act", "activation", reads=[("pX", b)], writes=[("ckvb", tb, c)], out=ckvb[tb][:, c, :],
                    in_=pX[b][:, :], func=AF.Copy)
                P.I("act", "activation", reads=[("pX", b)], writes=[("sqkvb", tb, c)], out=sqkvb[tb][:, c, :],
                    in_=pX[b][:, :], func=AF.Square)
            for (srcb, nchunk, dim, dst, key, skey) in ((sqb, 4, 512.0, fA, "fA", "sqb"),
                                                       (sqkvb, 2, 256.0, fB, "fB", "sqkvb")):
                b = nxt("pX", 2)
                P.MM([dict(out=pX[b][:, :], lhsT=ones_b[:, :], rhs=srcb[tb][:, c, :], start=(c == 0),
                           stop=(c == nchunk - 1)) for c in range(nchunk)],
                     reads=["ones_b"] + [(skey, tb, c) for c in range(nchunk)], writes=[("pX", b)])
                P.I("act", "activation", reads=[("pX", b)], writes=[(key, tb)], out=dst[tb][:, :], in_=pX[b][:, :],
                    func=AF.Sqrt, scale=1.0 / dim, bias=RMS_EPS)
                P.I("dve", "reciprocal", reads=[(key, tb)], writes=[(key, tb)], out=dst[tb][:, :], in_=dst[tb][:, :])
            P.MM([dict(out=pcol[:, sub:sub + 1], lhsT=sqkvb[tb][:, c, sub * 128:(sub + 1) * 128], rhs=ones_b[:, 0:1],
                       start=(c == 0), stop=(c == 1)) for sub in range(4) for c in range(2)],
                 reads=["ones_b", ("sqkvb", tb, 0), ("sqkvb", tb, 1)], writes=["pcol"])
            rkc = small[:, 16 + 4 * tb:20 + 4 * tb]
            P.I("act", "activation", reads=["pcol"], writes=[("rkvc", tb)], out=rkc, in_=pcol[:, 0:4], func=AF.Sqrt,
                scale=1.0 / 256.0, bias=RMS_EPS)
            P.I("dve", "reciprocal", reads=[("rkvc", tb)], writes=[("rkvc", tb)], out=rkc, in_=rkc)
            rope(lambda ps: [dict(out=ps[0:64, :], lhsT=Wa[:, k, 768:832], rhs=xTb[xb][:, k, :], start=(k == 0),
                                  stop=(k == 15)) for k in range(16)],
                 lambda ps: [dict(out=ps[0:64, :], lhsT=Wa[:, k, 832:896], rhs=xTb[xb][:, k, :], start=(k == 0),
                                  stop=(k == 15)) for k in range(16)],
                 ["Wa", ("xTb", xb)], tb, kr[0:64, tsl], [("kr", tt * 4 + c) for c in range(4)])
            rope(lambda ps: [dict(out=ps[0:64, :], lhsT=wqr[:, c, :], rhs=cqb[tb][:, c, :], start=(c == 0),
                                  stop=(c == 3)) for c in range(4)],
                 lambda ps: [dict(out=ps[0:64, :], lhsT=wqrs[:, c, :], rhs=cqb[tb][:, c, :], start=(c == 0),
                                  stop=(c == 3)) for c in range(4)],
                 ["wqr", "wqrs"] + [("cqb", tb, c) for c in range(4)], tb, qr[0:64, tsl], [("qr", tt)],
                 scale_key=("fA", tb), scale_t=fA[tb])
            b = nxt("pX", 2)
            P.MM([dict(out=pX[b][:, :], lhsT=wqn[:, c, :], rhs=cqb[tb][:, c, :], start=(c == 0), stop=(c == 3))
                  for c in range(4)], reads=["wqn"] + [("cqb", tb, c) for c in range(4)], writes=[("pX", b)])
            P.I("dve", "tensor_tensor", reads=[("pX", b), ("fA", tb)], writes=[("qn", tt)], out=qn[:, tsl],
                in0=pX[b][:, :], in1=fA[tb][:, :], op=ALU.mult)
            b = nxt("pX", 2)
            P.MM([dict(out=pX[b][:, :], lhsT=wkn[:, c, :], rhs=ckvb[tb][:, c, :], start=(c == 0), stop=(c == 1))
                  for c in range(2)], reads=["wkn", ("ckvb", tb, 0), ("ckvb", tb, 1)], writes=[("pX", b)])
            P.I("dve", "tensor_tensor", reads=[("pX", b), ("fB", tb)], writes=[("kn", tt * 4 + c) for c in range(4)],
                out=kn[:, tsl], in0=pX[b][:, :], in1=fB[tb][:, :], op=ALU.mult)
            for sub in range(4):
                b = nxt("pX", 2)
                P.MM([dict(out=pX[b][:, 0:128], lhsT=ckvb[tb][:, c, sub * 128:(sub + 1) * 128], rhs=wv[:, c, :],
                           start=(c == 0), stop=(c == 1)) for c in range(2)],
                     reads=["wv", ("ckvb", tb, 0), ("ckvb", tb, 1)], writes=[("pX", b)])
                P.I("dve", "tensor_scalar", reads=[("pX", b), ("rkvc", tb)], writes=[("Vh", tt * 4 + sub)],
                    out=Vh[:, tt * 4 + sub, :], in0=pX[b][:, 0:128],
                    scalar1=small[:, 16 + 4 * tb + sub:17 + 4 * tb + sub], scalar2=None, op0=ALU.mult)

        def mla_tile(hh, T):
            Slist = list(range(4 * T + 4))

            def score_specs(ps, S):
                return [dict(out=ps[:, :], lhsT=kn[:, S * 128:(S + 1) * 128], rhs=qn[:, T * 512:(T + 1) * 512],
                             start=True, stop=False),
                        dict(out=ps[:, :], lhsT=kr[0:64, S * 128:(S + 1) * 128], rhs=qr[0:64, T * 512:(T + 1) * 512],
                             start=False, stop=True)]

            def prob_fn(S, psb, ptb):
                P.I("act", "activation", reads=[("pS", psb)], writes=[("PT", ptb)], out=PT[ptb][:, :],
                    in_=pS[psb][:, :], func=AF.Exp, scale=SC_C)
                if S >= 4 * T:
                    P.I("pool", "tensor_tensor", reads=["mskC"], writes=[("PT", ptb)], out=PT[ptb][:, :],
                        in0=PT[ptb][:, :], in1=mskC[:, S - 4 * T, :], op=ALU.mult)

            def finish(po, pokey):
                ob = nxt("oT", 2)
                P.I("dve", "reciprocal", reads=["pD"], writes=[("fC", ob)], out=fC[ob][:, :], in_=pD[:, :])
                P.I("dve", "tensor_tensor", reads=[pokey, ("fC", ob)], writes=[("oT", ob)], out=oT[ob][:, :],
                    in0=po[:, :], in1=fC[ob][:, :], op=ALU.mult)
                P.dma("sp", yT[256 + hh * 128:256 + (hh + 1) * 128, T * 512:(T + 1) * 512], oT[ob][:, :],
                      reads=[("oT", ob)], semkey=("oT", ob), is_output=True)
            attention(Slist, score_specs, lambda S: [("kn", S), ("kr", S), ("qn", T), ("qr", T)], prob_fn,
                      lambda S: Vh[:, S, :], "Vh", 128, finish)

        for hh in range(2):
            P.barrier()
            mla_weights(hh)
            for tt in range(NT):
                mla_proj_tile(tt)
            for T in range(NT):
                mla_tile(hh, T)

    if do_B:
        P.barrier()
        mq, mk = B16[0], B16[1]
        mV = B16[2].rearrange("p (n d) -> p n d", d=128)
        raw = [P.sb([64, 515], F32) for _ in range(2)]
        acc = [P.sb([64, 512], F32) for _ in range(2)]
        rows = {nm: [P.sb([1, 512], F32) for _ in range(2)] for nm in ("b", "m")}
        for nm in ("i", "l", "lf"):
            _t = P.sb([1, 512], F32)
            rows[nm] = [_t, _t]
        rtmp = {nm: P.sb([1, 512], F32) for nm in ("fe", "g", "nm", "u")}
        ones_row = P.sb([1, 512], F32)
        ucol = P.sb([128, 32], F32)
        _g = P.sb([128, 512], F32)
        Gsb = [_g, _g]
        _e = P.sb([128, 512], F32)
        Esb = [_e, _e]
        P.dma("pool", Wa[:, :, 0:386], wB.rearrange("(k p) n -> p k n", p=128), writes=["Wa"])
        P.dma("sp", small[0:64, 24:32], cw_d[:, :], writes=["cw"])
        P.dma("sp", small[0:64, 32:34], cb_d[:, :], writes=["cb"])
        P.dma("sp", small[0:1, 34:36], gateb_d[:, :], writes=["gateb"])
        P.dma("sp", small[:, 36:37], mng_d[:, :], writes=["mng"])
        P.I("dve", "tensor_scalar", reads=["gateb"], writes=["negbf"], out=small[0:1, 37:38], in0=small[0:1, 35:36],
            scalar1=-1.0, scalar2=None, op0=ALU.mult)
        P.I("dve", "memset", writes=["ones_row"], ap=ones_row[:, :], constant=1.0)
        for qk in range(2):
            P.I("dve", "memset", writes=[("raw", qk)], ap=raw[qk][:, 0:3], constant=0.0)

        def b_pass1(tt):
            xb = load_x(tt)
            tsl = slice(tt * 512, (tt + 1) * 512)
            for qk, dst in ((0, mq), (1, mk)):
                b = proj_fm(xb, qk * 64, qk * 64 + 64, 64)
                if tt > 0:
                    P.I("pool", "tensor_copy", reads=[("raw", qk)], writes=[("raw", qk)], out=raw[qk][:, 0:3],
                        in_=raw[qk][:, 512:515])
                P.I("act", "activation", reads=[("pX", b)], writes=[("raw", qk)], out=raw[qk][:, 3:515],
                    in_=pX[b][0:64, :], func=AF.Copy)
                P.I("dve", "tensor_scalar", reads=[("raw", qk), "cw"], writes=[("acc", qk)], out=acc[qk][:, :],
                    in0=raw[qk][:, 0:512], scalar1=small[0:64, 24 + 4 * qk:25 + 4 * qk], scalar2=None, op0=ALU.mult)
                for j in range(1, 4):
                    P.I("dve", "scalar_tensor_tensor", reads=[("raw", qk), "cw"], writes=[("acc", qk)],
                        out=acc[qk][:, :], in0=raw[qk][:, j:j + 512],
                        scalar=small[0:64, 24 + 4 * qk + j:25 + 4 * qk + j], in1=acc[qk][:, :], op0=ALU.mult,
                        op1=ALU.add)
                P.I("act", "activation", reads=[("acc", qk), "cb"],
                    writes=[("mq", tt)] if qk == 0 else [("mk", tt * 4 + c) for c in range(4)],
                    out=dst[0:64, tsl], in_=acc[qk][:, :], func=AF.Silu, bias=small[0:64, 32 + qk:33 + qk], scale=1.0)
            for sub in range(4):
                b = proj_tm(xb, sub, 128, 256)
                copy_out("dve" if sub % 2 else "act", mV[:, tt * 4 + sub, :], pX[b][:, 0:128], [("pX", b)],
                         [("mV", tt * 4 + sub)])

        def b_tile(T):
            xb = load_x(T)
            rb = T % 2
            pb = (T + 1) % 2
            R = {k: v[rb] for k, v in rows.items()}
            bi = proj_fm(xb, 384, 385, 1)
            P.I("dve", "tensor_scalar", reads=[("pX", bi), "gateb"], writes=[("row_i", 0)], out=R["i"][:, :],
                in0=pX[bi][0:1, :], scalar1=small[0:1, 34:35], scalar2=None, op0=ALU.add)
            bf = proj_fm(xb, 385, 386, 1)
            P.I("act", "activation", reads=[("pX", bf), "negbf"], writes=["row_fe"], out=rtmp["fe"][:, :],
                in_=pX[bf][0:1, :], func=AF.Exp, bias=small[0:1, 37:38], scale=-1.0)
            P.I("act", "activation", reads=["row_fe"], writes=[("row_l", 0)], out=R["l"][:, :], in_=rtmp["fe"][:, :],
                func=AF.Ln, bias=1.0, scale=1.0)
            P.I("dve", "tensor_scalar", reads=[("row_l", 0)], writes=[("row_lf", 0)], out=R["lf"][:, :],
                in0=R["l"][:, :], scalar1=-1.0, scalar2=None, op0=ALU.mult)
            init_b = 0.0 if T == 0 else rows["b"][pb][:, 511:512]
            init_m = 0.0 if T == 0 else rows["m"][pb][:, 511:512]
            P.I("dve", "tensor_tensor_scan", reads=["ones_row", ("row_l", 0), ("row_b", pb)], writes=[("row_b", rb)],
                out=R["b"][:, :], data0=ones_row[:, :], data1=R["l"][:, :], initial=init_b, op0=ALU.mult,
                op1=ALU.subtract)
            P.I("dve", "tensor_tensor_scan", reads=[("row_lf", 0), ("row_i", 0), ("row_m", pb)],
                writes=[("row_m", rb)], out=R["m"][:, :], data0=R["lf"][:, :], data1=R["i"][:, :], initial=init_m,
                op0=ALU.add, op1=ALU.max)
            P.I("dve", "tensor_tensor", reads=[("row_b", rb), ("row_m", rb)], writes=["row_g"], out=rtmp["g"][:, :],
                in0=R["b"][:, :], in1=R["m"][:, :], op=ALU.subtract)
            P.I("dve", "tensor_scalar", reads=[("row_m", rb)], writes=["row_nm"], out=rtmp["nm"][:, :],
                in0=R["m"][:, :], scalar1=-1.0, scalar2=None, op0=ALU.mult)
            P.I("dve", "tensor_tensor", reads=[("row_i", 0), ("row_b", rb)], writes=["row_u"], out=rtmp["u"][:, :],
                in0=R["i"][:, :], in1=R["b"][:, :], op=ALU.subtract)
            b = nxt("pX", 2)
            P.MM([dict(out=pX[b][:, :], lhsT=ones_f[0:1, :], rhs=rtmp["g"][0:1, :], start=True, stop=True)],
                 reads=["ones_f", "row_g"], writes=[("pX", b)])
            copy_out("act", Gsb[rb][:, :], pX[b][:, :], [("pX", b)], [("Gsb", 0)])
            b = nxt("pX", 2)
            P.MM([dict(out=pX[b][:, :], lhsT=ones_f[0:1, :], rhs=rtmp["nm"][0:1, :], start=True, stop=True)],
                 reads=["ones_f", "row_nm"], writes=[("pX", b)])
            P.I("act", "activation", reads=[("pX", b)], writes=[("Esb", 0)], out=Esb[rb][:, :], in_=pX[b][:, :],
                func=AF.Exp)
            P.MM([dict(out=pcol[:, c:c + 1], lhsT=rtmp["u"][0:1, c * 128:(c + 1) * 128], rhs=ones_f[0:1, 0:1],
                       start=True, stop=True) for c in range(4)], reads=["ones_f", "row_u"], writes=["pcol"])
            copy_out("dve", ucol[:, 4 * T:4 * T + 4], pcol[:, 0:4], ["pcol"], [("ucol", 4 * T + c) for c in range(4)])

            Slist = list(range(4 * T + 4))

            def score_specs(ps, S):
                return [dict(out=ps[:, :], lhsT=mk[0:64, S * 128:(S + 1) * 128], rhs=mq[0:64, T * 512:(T + 1) * 512],
                             start=True, stop=True)]

            def prob_fn(S, psb, ptb):
                wb = nxt("Wt", 2)
                P.I("act", "activation", reads=[("Gsb", 0), ("ucol", S)], writes=[("Wt", wb)], out=Wt[wb][:, :],
                    in_=Gsb[rb][:, :], func=AF.Exp, bias=ucol[:, S:S + 1], scale=1.0)
                P.I("dve", "scalar_tensor_tensor", reads=[("pS", psb), ("Wt", wb)], writes=[("PT", ptb)],
                    out=PT[ptb][:, :], in0=pS[psb][:, :], scalar=SC_B, in1=Wt[wb][:, :], op0=ALU.mult, op1=ALU.mult)
                if S >= 4 * T:
                    P.I("pool", "tensor_tensor", reads=["mskC"], writes=[("PT", ptb)], out=PT[ptb][:, :],
                        in0=PT[ptb][:, :], in1=mskC[:, S - 4 * T, :], op=ALU.mult)

            def finish(po, pokey):
                ob = nxt("oT", 2)
                P.I("act", "activation", reads=["pD"], writes=[("fA", ob)], out=fA[ob][:, :], in_=pD[:, :],
                    func=AF.Abs)
                P.I("dve", "tensor_tensor", reads=[("fA", ob), ("Esb", 0)], writes=[("fA", ob)], out=fA[ob][:, :],
                    in0=fA[ob][:, :], in1=Esb[rb][:, :], op=ALU.max)
                P.I("dve", "reciprocal", reads=[("fA", ob)], writes=[("fA", ob)], out=fA[ob][:, :], in_=fA[ob][:, :])
                P.I("dve", "tensor_tensor", reads=[pokey, ("fA", ob)], writes=[("fB", ob)], out=fB[ob][:, :],
                    in0=po[:, :], in1=fA[ob][:, :], op=ALU.mult)
                bo = proj_fm(xb, 256, 384, 128)
                P.I("act", "activation", reads=[("pX", bo)], writes=[("fA", ob)], out=fA[ob][:, :], in_=pX[bo][:, :],
                    func=AF.Sigmoid)
                P.I("dve", "tensor_tensor", reads=[("fA", ob)], writes=[("fB", ob)], out=fB[ob][:, :],
                    in0=fB[ob][:, :], in1=fA[ob][:, :], op=ALU.mult)
                b1 = nxt("pX", 2)
                P.MM([dict(out=pX[b1][:, :], lhsT=onesdiv_f[:, :], rhs=fB[ob][:, :], start=True, stop=True)],
                     reads=["onesdiv_f", ("fB", ob)], writes=[("pX", b1)])
                P.I("dve", "tensor_tensor", reads=[("pX", b1)], writes=[("fB", ob)], out=fB[ob][:, :],
                    in0=fB[ob][:, :], in1=pX[b1][:, :], op=ALU.subtract)
                P.I("pool", "tensor_tensor", reads=[("fB", ob)], writes=[("fC", ob)], out=fC[ob][:, :],
                    in0=fB[ob][:, :], in1=fB[ob][:, :], op=ALU.mult)
                b2 = nxt("pX", 2)
                P.MM([dict(out=pX[b2][:, :], lhsT=onesdiv_f[:, :], rhs=fC[ob][:, :], start=True, stop=True)],
                     reads=["onesdiv_f", ("fC", ob)], writes=[("pX", b2)])
                P.I("act", "activation", reads=[("pX", b2)], writes=[("fC", ob)], out=fC[ob][:, :], in_=pX[b2][:, :],
                    func=AF.Sqrt, bias=LNH_EPS, scale=1.0)
                P.I("dve", "reciprocal", reads=[("fC", ob)], writes=[("fC", ob)], out=fC[ob][:, :], in_=fC[ob][:, :])
                P.I("dve", "scalar_tensor_tensor", reads=[("fB", ob), ("fC", ob), "mng"], writes=[("oT", ob)],
                    out=oT[ob][:, :], in0=fB[ob][:, :], scalar=small[:, 36:37], in1=fC[ob][:, :], op0=ALU.mult,
                    op1=ALU.mult)
                P.dma("sp", yT[128:256, T * 512:(T + 1) * 512], oT[ob][:, :], reads=[("oT", ob)], semkey=("oT", ob),
                      is_output=True)
            attention(Slist, score_specs, lambda S: [("mk", S), ("mq", T)], prob_fn, lambda S: mV[:, S, :], "mV", 128,
                      finish)

        for tt in range(NT):
            b_pass1(tt)
        for T in range(NT):
            b_tile(T)
    return P.emit()


def host_consts_M():
    S = 4096
    inv = 1.0 / (10000.0 ** (np.arange(0, 64, 2, dtype=np.float32) / 64))
    ang = np.arange(S, dtype=np.float32)[:, None] * inv[None, :]
    cos, sin = np.cos(ang).astype(np.float32), np.sin(ang).astype(np.float32)
    cosT = np.concatenate([cos.T, cos.T], 0)
    sinT = np.concatenate([-sin.T, sin.T], 0)
    kk = np.arange(128)[:, None]
    qq = np.arange(512)[None, :]
    maskC = np.stack([(qq >= d * 128 + kk) for d in range(4)]).astype(np.float32).reshape(4 * 128, 512)
    mA = []
    for r in range(5):
        rel = qq - (r - 1) * 128 - kk
        mA.append(((rel >= 0) & (rel < 128)).astype(np.float32))
    maskA = np.stack(mA).reshape(5 * 128, 512)
    import ml_dtypes
    return {"cosT": np.ascontiguousarray(cosT), "sinT": np.ascontiguousarray(sinT),
            "maskA": maskA.astype(ml_dtypes.bfloat16), "maskC": maskC.astype(ml_dtypes.bfloat16)}


def host_inputs_M(j, xTb_f32, w_in, conv_w, conv_b, m_gate_b, m_norm_g, sinks, q_norm_g, w_uq, kv_norm_g, w_ukv):
    A_Q, A_KV, B_QK, B_V = 512, 128, 256, 512
    o = {}
    off = 0
    a_q = w_in[:, off:off + A_Q]; off += A_Q
    a_k = w_in[:, off:off + A_KV]; off += A_KV
    a_v = w_in[:, off:off + A_KV]; off += A_KV
    m_q = w_in[:, off:off + B_QK]; off += B_QK
    m_k = w_in[:, off:off + B_QK]; off += B_QK
    m_v = w_in[:, off:off + B_V]; off += B_V
    m_o = w_in[:, off:off + B_V]; off += B_V
    m_if = w_in[:, off:off + 8]; off += 8
    c_q = w_in[:, off:off + 512]; off += 512
    c_kv = w_in[:, off:off + 256]; off += 256
    c_kr = w_in[:, off:off + 64]; off += 64
    kvh = j // 2
    o["xT"] = xTb_f32
    o["wA"] = np.ascontiguousarray(np.concatenate(
        [a_q[:, (2 * j) * 64:(2 * j + 2) * 64], a_k[:, kvh * 64:(kvh + 1) * 64], a_v[:, kvh * 64:(kvh + 1) * 64]], 1))
    o["wB"] = np.ascontiguousarray(np.concatenate(
        [m_q[:, j * 64:(j + 1) * 64], m_k[:, j * 64:(j + 1) * 64], m_v[:, j * 128:(j + 1) * 128],
         m_o[:, j * 128:(j + 1) * 128], m_if[:, j:j + 1], m_if[:, 4 + j:5 + j]], 1))
    o["wC"] = np.ascontiguousarray(np.concatenate([c_q, c_kv, c_kr, c_kr[:, 32:64], c_kr[:, 0:32]], 1))
    uq = w_uq.reshape(512, 8, 192)
    blocks = []
    for h in (2 * j, 2 * j + 1):
        blocks += [uq[:, h, 0:128], uq[:, h, 128:192], uq[:, h, 160:192], uq[:, h, 128:160]]
    o["wuq"] = np.ascontiguousarray(np.concatenate(blocks, 1))
    ukv = w_ukv.reshape(256, 8, 256)
    o["wukv"] = np.ascontiguousarray(np.concatenate([ukv[:, 2 * j, :], ukv[:, 2 * j + 1, :]], 1))
    o["gq_col"] = np.ascontiguousarray(q_norm_g.reshape(4, 128).T)
    o["gkv_col"] = np.ascontiguousarray(kv_norm_g.reshape(2, 128).T)
    cwq = conv_w[:, j * 64:(j + 1) * 64].T
    cwk = conv_w[:, 256 + j * 64:256 + (j + 1) * 64].T
    o["cw"] = np.ascontiguousarray(np.concatenate([cwq, cwk], 1))
    o["cb"] = np.ascontiguousarray(np.stack([conv_b[j * 64:(j + 1) * 64], conv_b[256 + j * 64:256 + (j + 1) * 64]], 1))
    o["gateb"] = np.ascontiguousarray(np.array([[m_gate_b[j], m_gate_b[4 + j]]], np.float32))
    o["mng"] = np.ascontiguousarray(m_norm_g[j * 128:(j + 1) * 128].reshape(128, 1))
    o["sinks"] = np.ascontiguousarray(np.tile(sinks[2 * j:2 * j + 2][None, :], (64, 1)))
    return o


_PROGS = {}


def _prog(name, builder):
    if name not in _PROGS:
        nc = bass.Bass("TRN2", target_bir_lowering=False)
        builder(nc)
        _PROGS[name] = nc
    return _PROGS[name]


def _run(nc, in_maps):
    res = run_bass_kernel_spmd(nc, in_maps, core_ids=list(range(8)))
    return res.results


def kernel(x, w_in, conv_w, conv_b, m_gate_b, m_norm_g, sinks, q_norm_g, w_uq, kv_norm_g, w_ukv, w_out,
           ln1_g, ln1_b, w_router, b_router, w_gate_up, b_gate_up, w_down, b_down, ln2_g, ln2_b):
    f32 = np.float32
    x = np.asarray(x, f32)
    ncM = _prog("M", build_M)
    ncO = _prog("O", build_O)
    ncE = _prog("E", build_E)
    ncN = _prog("N", build_N)
    cM = host_consts_M()
    cO = host_consts_O()
    cE = host_consts_E()
    tile128 = lambda v: np.ascontiguousarray(np.broadcast_to(np.asarray(v, f32)[None, :], (128, v.shape[0])))
    for l in range(2):
        xT = [np.ascontiguousarray(x[b].T) for b in range(2)]
        maps = []
        for c in range(8):
            b, j = c // 4, c % 4
            m = host_inputs_M(j, xT[b], np.asarray(w_in[l]), np.asarray(conv_w[l]), np.asarray(conv_b[l]),
                              np.asarray(m_gate_b[l]), np.asarray(m_norm_g[l]), np.asarray(sinks[l]),
                              np.asarray(q_norm_g[l]), np.asarray(w_uq[l]), np.asarray(kv_norm_g[l]),
                              np.asarray(w_ukv[l]))
            m.update(cM)
            maps.append(m)
        rM = _run(ncM, maps)
        yT = [np.empty((2048, 4096), f32) for _ in range(2)]
        for c in range(8):
            b, j = c // 4, c % 4
            y = rM[c]["yT"]
            yT[b][128 * j:128 * j + 128] = y[0:128]
            yT[b][512 + 128 * j:512 + 128 * j + 128] = y[128:256]
            yT[b][1024 + 256 * j:1024 + 256 * j + 256] = y[256:512]
        del rM, maps
        g1, b1 = tile128(ln1_g[l]), tile128(ln1_b[l])
        br = tile128(b_router[l])
        wo = np.ascontiguousarray(w_out[l], f32)
        wr = np.ascontiguousarray(w_router[l], f32)
        maps = []
        for c in range(8):
            b, i = c // 4, c % 4
            m = {"yT": np.ascontiguousarray(yT[b][:, i * 1024:(i + 1) * 1024]),
                 "xres": np.ascontiguousarray(x[b, i * 1024:(i + 1) * 1024]),
                 "wout": wo, "g1": g1, "b1": b1, "wr": wr, "br": br}
            m.update(cO)
            maps.append(m)
        rO = _run(ncO, maps)
        x1 = [rO[c]["x1"] for c in range(8)]
        x1b_all = np.concatenate([rO[c]["x1b"] for c in range(8)], 0)
        G_all = np.concatenate([rO[c]["G"] for c in range(8)], 0)
        del rO, maps, yT
        maps = []
        for c in range(8):
            e0 = 4 * c
            m = {"x1b": x1b_all, "Gc": np.ascontiguousarray(G_all[:, e0:e0 + 4]),
                 "wgu": np.asarray(w_gate_up[l, e0:e0 + 4]).reshape(4 * 2048, 4096),
                 "bgut": np.ascontiguousarray(
                     np.asarray(b_gate_up[l, e0:e0 + 4]).reshape(4, 32, 128).transpose(0, 2, 1)).reshape(4 * 128, 32),
                 "wd": np.asarray(w_down[l, e0:e0 + 4]).reshape(4 * 2048, 2048),
                 "bdr": np.ascontiguousarray(
                     np.broadcast_to(np.asarray(b_down[l, e0:e0 + 4])[:, None, :], (4, 128, 2048))).reshape(4 * 128, 2048)}
            m.update(cE)
            maps.append(m)
        rE = _run(ncE, maps)
        parts = [rE[c]["part"] for c in range(8)]
        del rE, maps, x1b_all
        g2, b2 = tile128(ln2_g[l]), tile128(ln2_b[l])
        maps = []
        for c in range(8):
            pc = np.concatenate([parts[s][c * 1024:(c + 1) * 1024] for s in range(8)], 0)
            maps.append({"parts": pc, "x1": x1[c], "g2": g2, "b2": b2})
        rN = _run(ncN, maps)
        x = np.concatenate([rN[c]["x2"] for c in range(8)], 0).reshape(2, 4096, 2048)
        del rN, maps, parts, x1
    return np.ascontiguousarray(x, dtype=f32)
```
